# Optimizing a Trainium2 kernel written in Bass

```python
import math
import jax, jax.numpy as jnp
from jax import lax
import numpy as np

D_MODEL = 2048
BATCH = 4
SEQ = 2048
DEPTH = 4

FOX_HEAD_DIM = 128
FOX_HEADS = D_MODEL // FOX_HEAD_DIM
FOX_WIDTH = FOX_HEADS * FOX_HEAD_DIM
Q_BLOCK = 128
FOX_FORGET_BIAS = 3.0
GDN_HEAD_DIM = 128
GDN_K_HEADS = D_MODEL // GDN_HEAD_DIM
GDN_V_HEADS = 2 * GDN_K_HEADS
GDN_K_WIDTH = GDN_K_HEADS * GDN_HEAD_DIM
GDN_V_WIDTH = GDN_V_HEADS * GDN_HEAD_DIM
GDN_CONV_CH = 2 * GDN_K_WIDTH + GDN_V_WIDTH
GDN_CONV = 4
GDN_CHUNK = 64
FFN_DIM = 5632
N_EXPERTS = 8
TOP_K = 2
EXPERT_DIM = FFN_DIM // 2
N_FOX_LAYERS = (DEPTH + 1) // 2
N_GDN_LAYERS = DEPTH // 2
EPS = 1e-6
F32 = jnp.float32

kernel_name = 'hybrid_fox_gdn_moe_trunk'


def rmsnorm(x, g):
    xf = x.astype(F32)
    y = xf * lax.rsqrt(jnp.mean(xf * xf, axis=-1, keepdims=True) + EPS)
    return (y * g.astype(F32)).astype(x.dtype)


def l2norm(x):
    xf = x.astype(F32)
    return xf * lax.rsqrt(jnp.sum(xf * xf, axis=-1, keepdims=True) + EPS)


def swiglu(t, w_gate, w_up, w_down):
    return (jax.nn.silu(t @ w_gate) * (t @ w_up)) @ w_down


def fox_attention(h, w_in, b_f, q_gain, k_gain, w_out):
    B, S, _ = h.shape
    proj = h @ w_in
    q, k, v, og, f_logit = jnp.split(
        proj, [FOX_WIDTH, 2 * FOX_WIDTH, 3 * FOX_WIDTH, 4 * FOX_WIDTH], axis=-1)
    q = rmsnorm(q.reshape(B, S, FOX_HEADS, FOX_HEAD_DIM), q_gain).transpose(0, 2, 1, 3)
    k = rmsnorm(k.reshape(B, S, FOX_HEADS, FOX_HEAD_DIM), k_gain).transpose(0, 2, 1, 3)
    v = v.reshape(B, S, FOX_HEADS, FOX_HEAD_DIM).transpose(0, 2, 1, 3)
    log_f = jax.nn.log_sigmoid((f_logit + b_f).astype(F32))
    cum = jnp.cumsum(log_f, axis=1).transpose(0, 2, 1)
    scale = FOX_HEAD_DIM ** -0.5
    outs = []
    for blk in range(S // Q_BLOCK):
        lo, hi = blk * Q_BLOCK, (blk + 1) * Q_BLOCK
        s = jnp.einsum('bhqd,bhkd->bhqk', q[:, :, lo:hi], k[:, :, :hi]).astype(F32) * scale
        s = s + cum[:, :, lo:hi, None] - cum[:, :, None, :hi]
        causal = (lo + jnp.arange(Q_BLOCK))[:, None] >= jnp.arange(hi)[None, :]
        p = jax.nn.softmax(jnp.where(causal, s, -jnp.inf), axis=-1)
        outs.append(jnp.einsum('bhqk,bhkd->bhqd', p.astype(v.dtype), v[:, :, :hi]))
    o = jnp.concatenate(outs, axis=2).transpose(0, 2, 1, 3).reshape(B, S, FOX_WIDTH)
    return (o * jax.nn.sigmoid(og)) @ w_out


def causal_conv(x, w):
    K, C = w.shape
    return lax.conv_general_dilated(
        x, w[:, None, :].astype(x.dtype), window_strides=(1,), padding=[(K - 1, 0)],
        dimension_numbers=('NWC', 'WIO', 'NWC'), feature_group_count=C)


def gated_delta_rule(q, k, v, g, beta):
    B, S, H, DK = q.shape
    DV = v.shape[-1]
    C = GDN_CHUNK
    N = S // C

    def chunks(t):
        return jnp.moveaxis(t, 2, 1).reshape((B, H, N, C) + t.shape[3:])

    q = chunks(q) * DK ** -0.5
    k = chunks(k)
    v = chunks(v)
    beta = chunks(beta)
    g = jnp.cumsum(chunks(g), axis=-1)
    incl = jnp.tril(jnp.ones((C, C), dtype=bool))
    strict = jnp.tril(jnp.ones((C, C), dtype=bool), -1)
    decay = jnp.exp(jnp.where(incl, g[..., :, None] - g[..., None, :], -jnp.inf))
    kb = k * beta[..., None]
    vb = v * beta[..., None]
    lmat = jnp.where(strict, jnp.einsum('bhnid,bhnjd->bhnij', kb, k) * decay, 0.0)
    eye = jnp.eye(C, dtype=F32)
    rhs = jnp.concatenate([vb, kb * jnp.exp(g)[..., None]], axis=-1)
    sol = lax.linalg.triangular_solve(eye + lmat, rhs, left_side=True, lower=True,
                                      unit_diagonal=True)
    u, w = sol[..., :DV], sol[..., DV:]
    intra = jnp.where(incl, jnp.einsum('bhnid,bhnjd->bhnij', q, k) * decay, 0.0)

    def step(state, xs):
        q_c, k_c, u_c, w_c, a_c, g_c = xs
        v_new = u_c - jnp.einsum('bhcd,bhde->bhce', w_c, state)
        o = (jnp.einsum('bhcd,bhde->bhce', q_c * jnp.exp(g_c)[..., None], state)
             + jnp.einsum('bhij,bhje->bhie', a_c, v_new))
        g_last = g_c[..., -1]
        k_dec = k_c * jnp.exp(g_last[..., None] - g_c)[..., None]
        state = (state * jnp.exp(g_last)[..., None, None]
                 + jnp.einsum('bhcd,bhce->bhde', k_dec, v_new))
        return state, o

    xs = tuple(jnp.moveaxis(t, 2, 0) for t in (q, k, u, w, intra, g))
    state0 = jnp.zeros((B, H, DK, DV), F32)
    _, o = lax.scan(step, state0, xs)
    return jnp.moveaxis(o, 0, 2).reshape(B, H, S, DV).transpose(0, 2, 1, 3)


def gdn_mixer(h, w_in, conv_w, a_log, dt_bias, o_gain, w_out):
    B, S, _ = h.shape
    proj = h @ w_in
    qkv, z, b, a = jnp.split(
        proj, [GDN_CONV_CH, GDN_CONV_CH + GDN_V_WIDTH, GDN_CONV_CH + GDN_V_WIDTH + GDN_V_HEADS],
        axis=-1)
    qkv = jax.nn.silu(causal_conv(qkv, conv_w))
    q, k, v = jnp.split(qkv, [GDN_K_WIDTH, 2 * GDN_K_WIDTH], axis=-1)
    rep = GDN_V_HEADS // GDN_K_HEADS
    q = jnp.repeat(l2norm(q.reshape(B, S, GDN_K_HEADS, GDN_HEAD_DIM)), rep, axis=2)
    k = jnp.repeat(l2norm(k.reshape(B, S, GDN_K_HEADS, GDN_HEAD_DIM)), rep, axis=2)
    v = v.reshape(B, S, GDN_V_HEADS, GDN_HEAD_DIM).astype(F32)
    beta = jax.nn.sigmoid(b.astype(F32))
    g = -jnp.exp(a_log.astype(F32)) * jax.nn.softplus(a.astype(F32) + dt_bias.astype(F32))
    o = gated_delta_rule(q, k, v, g, beta).astype(h.dtype)
    o = rmsnorm(o, o_gain) * jax.nn.silu(z.reshape(B, S, GDN_V_HEADS, GDN_HEAD_DIM))
    return o.reshape(B, S, GDN_V_WIDTH) @ w_out


def moe_ffn(h, router, w_gate, w_up, w_down):
    B, S, D = h.shape
    t = h.reshape(B * S, D)
    logits = (t @ router).astype(F32)
    top_v, top_i = lax.top_k(logits, TOP_K)
    wts = jax.nn.softmax(top_v, axis=-1)
    gates = jnp.sum(jax.nn.one_hot(top_i, N_EXPERTS, dtype=F32) * wts[..., None], axis=1)
    out = jnp.zeros_like(t)
    for e in range(N_EXPERTS):
        out = out + gates[:, e:e + 1].astype(t.dtype) * swiglu(t, w_gate[e], w_up[e], w_down[e])
    return out.reshape(B, S, D)


def setup_inputs(seed: int = 0) -> dict:
    key = jax.random.key(seed)
    ks = jax.random.split(key, 21)
    NA, NB = N_FOX_LAYERS, N_GDN_LAYERS
    fox_in = 4 * FOX_WIDTH + FOX_HEADS
    gdn_in = GDN_CONV_CH + GDN_V_WIDTH + 2 * GDN_V_HEADS
    out_gain = 0.5

    def nrm(k, shape, fan_in, gain=1.0):
        return gain * fan_in ** -0.5 * jax.random.normal(k, shape, F32)

    def gain(k, shape):
        return 1.0 + 0.02 * jax.random.normal(k, shape, F32)

    dt = jnp.exp(jax.random.uniform(ks[11], (NB, GDN_V_HEADS), F32,
                                    math.log(1e-3), math.log(1e-1)))
    return {
        'x': jax.random.normal(ks[0], (BATCH, SEQ, D_MODEL), F32),
        'norm_mix': gain(ks[1], (DEPTH, D_MODEL)),
        'norm_ffn': gain(ks[2], (DEPTH, D_MODEL)),
        'fox_w_in': nrm(ks[3], (NA, D_MODEL, fox_in), D_MODEL),
        'fox_b_f': FOX_FORGET_BIAS + 0.5 * jax.random.normal(ks[4], (NA, FOX_HEADS), F32),
        'fox_q_norm': gain(ks[5], (NA, FOX_HEAD_DIM)),
        'fox_k_norm': gain(ks[6], (NA, FOX_HEAD_DIM)),
        'fox_w_out': nrm(ks[7], (NA, FOX_WIDTH, D_MODEL), FOX_WIDTH, out_gain),
        'gdn_w_in': nrm(ks[8], (NB, D_MODEL, gdn_in), D_MODEL),
        'gdn_conv': nrm(ks[9], (NB, GDN_CONV, GDN_CONV_CH), GDN_CONV),
        'gdn_a_log': jnp.log(jax.random.uniform(ks[10], (NB, GDN_V_HEADS), F32, 1.0, 16.0)),
        'gdn_dt_bias': jnp.log(jnp.expm1(dt)),
        'gdn_o_norm': gain(ks[12], (NB, GDN_HEAD_DIM)),
        'gdn_w_out': nrm(ks[13], (NB, GDN_V_WIDTH, D_MODEL), GDN_V_WIDTH, out_gain),
        'ffn_w_gate': nrm(ks[14], (NA, D_MODEL, FFN_DIM), D_MODEL),
        'ffn_w_up': nrm(ks[15], (NA, D_MODEL, FFN_DIM), D_MODEL),
        'ffn_w_down': nrm(ks[16], (NA, FFN_DIM, D_MODEL), FFN_DIM, out_gain),
        'moe_router': nrm(ks[17], (NB, D_MODEL, N_EXPERTS), D_MODEL),
        'moe_w_gate': nrm(ks[18], (NB, N_EXPERTS, D_MODEL, EXPERT_DIM), D_MODEL),
        'moe_w_up': nrm(ks[19], (NB, N_EXPERTS, D_MODEL, EXPERT_DIM), D_MODEL),
        'moe_w_down': nrm(ks[20], (NB, N_EXPERTS, EXPERT_DIM, D_MODEL), EXPERT_DIM, out_gain),
    }


def reference(x, norm_mix, norm_ffn, fox_w_in, fox_b_f, fox_q_norm, fox_k_norm, fox_w_out,
              gdn_w_in, gdn_conv, gdn_a_log, gdn_dt_bias, gdn_o_norm, gdn_w_out,
              ffn_w_gate, ffn_w_up, ffn_w_down,
              moe_router, moe_w_gate, moe_w_up, moe_w_down):
    h = x
    for i in range(DEPTH):
        j = i // 2
        hn = rmsnorm(h, norm_mix[i])
        if i % 2 == 0:
            h = h + fox_attention(hn, fox_w_in[j], fox_b_f[j], fox_q_norm[j], fox_k_norm[j],
                                  fox_w_out[j])
            h = h + swiglu(rmsnorm(h, norm_ffn[i]), ffn_w_gate[j], ffn_w_up[j], ffn_w_down[j])
        else:
            h = h + gdn_mixer(hn, gdn_w_in[j], gdn_conv[j], gdn_a_log[j], gdn_dt_bias[j],
                              gdn_o_norm[j], gdn_w_out[j])
            h = h + moe_ffn(rmsnorm(h, norm_ffn[i]), moe_router[j], moe_w_gate[j],
                            moe_w_up[j], moe_w_down[j])
    return h
```

```python
import contextlib
import numpy as np
import concourse.bass as bass
import concourse.mybir as mybir
from concourse.bass_utils import run_bass_kernel_spmd

F32 = mybir.dt.float32
BF16 = mybir.dt.bfloat16
AF = mybir.ActivationFunctionType
ALU = mybir.AluOpType
AX = mybir.AxisListType


class Tile:
    def __init__(self, ap, name=""):
        self.ap = ap
        self.name = name
        self.last_write = None
        self.readers = {}
        self.dma_sem = None
        self.dma_count = 0

    def __getitem__(self, idx):
        return self.ap[idx]


class Eng:
    def __init__(self, name, obj, sem):
        self.name = name
        self.obj = obj
        self.sem = sem
        self.count = 0
        self.waited = {}


class Ctx:
    SAME_ENGINE_SYNC = True

    def __init__(self, nc, stack):
        self.nc = nc
        self.stack = stack
        self.root = stack
        mk = lambda n: stack.enter_context(nc.semaphore(n))
        self.pe = Eng("pe", nc.tensor, mk("s_pe"))
        self.act = Eng("act", nc.scalar, mk("s_act"))
        self.dve = Eng("dve", nc.vector, mk("s_dve"))
        self.pool = Eng("pool", nc.gpsimd, mk("s_pool"))
        self.sp = Eng("sp", nc.sync, mk("s_sp"))
        self.engs = [self.pe, self.act, self.dve, self.pool, self.sp]
        self.n_sems = 5
        self._uid = 0

    def sbuf(self, shape, dtype, name=None):
        self._uid += 1
        name = f"sb{self._uid}_{name or ''}"
        t = self.stack.enter_context(self.nc.sbuf_tensor(name, list(shape), dtype))
        return t

    def psum(self, shape, dtype=F32, name=None):
        self._uid += 1
        name = f"ps{self._uid}_{name or ''}"
        t = self.stack.enter_context(self.nc.psum_tensor(name, list(shape), dtype))
        return t

    def tile(self, ap, name=""):
        return Tile(ap, name)

    def stile(self, shape, dtype, name=None):
        t = self.sbuf(shape, dtype, name)
        return Tile(t[:] if False else t, name or "")

    def _wait(self, eng, ticket):
        if ticket is None:
            return
        sem, val = ticket
        if sem is eng.sem and (eng is self.pe or not self.SAME_ENGINE_SYNC or val > eng.count):
            return
        key = id(sem)
        if eng.waited.get(key, 0) >= val:
            return
        eng.obj.wait_ge(sem, val)
        eng.waited[key] = val

    def _deps(self, eng, reads, writes):
        for t in reads:
            self._wait(eng, t.last_write)
        for t in writes:
            self._wait(eng, t.last_write)
            for tk in list(t.readers.values()):
                self._wait(eng, tk)

    def _record(self, ticket, reads, writes):
        for t in reads:
            sem, val = ticket
            old = t.readers.get(id(sem))
            if old is None or old[1] < val:
                t.readers[id(sem)] = ticket
        for t in writes:
            t.last_write = ticket
            t.readers = {}

    def op(self, eng, fn, reads=(), writes=(), signal=True):
        self._deps(eng, reads, writes)
        ins = fn(eng.obj)
        if signal:
            ins.then_inc(eng.sem, 1)
            eng.count += 1
            ticket = (eng.sem, eng.count)
        else:
            ticket = (eng.sem, eng.count + 1)
        self._record(ticket, reads, writes)
        return ticket

    def dma(self, eng, out_ap, in_ap, dst=None, src=None, **kw):
        owner = dst if dst is not None else src
        if owner.dma_sem is None:
            owner.dma_sem = self.root.enter_context(self.nc.semaphore(f"d{self.n_sems}"))
            self.n_sems += 1
        reads = [src] if src is not None else []
        writes = [dst] if dst is not None else []
        self._deps(eng, reads, writes)
        ins = eng.obj.dma_start(out=out_ap, in_=in_ap, **kw)
        ins.then_inc(owner.dma_sem, 16)
        owner.dma_count += 16
        ticket = (owner.dma_sem, owner.dma_count)
        self._record(ticket, reads, writes)
        return ticket

    def finish(self, tiles):
        for t in tiles:
            self._wait(self.sp, t.last_write)
            for tk in t.readers.values():
                self._wait(self.sp, tk)

D = 2048
S = 2048
NK = 16
EPS = 1e-6


T = 1024
SGC = 11
EPS = 1e-6


def build_ffn(KO, E, FH):
    KOC = KO // 128
    NFC = FH // 128
    NS = NFC // SGC
    nc = bass.Bass("TRN2", target_bir_lowering=False)
    dr = lambda n, s, dt=F32: nc.dram_tensor(n, list(s), dt, kind="ExternalInput").ap()
    hT_d = dr("hT", [128, NK, T])
    oT_d = dr("oT", [128, KOC, T], BF16)
    wo_d = dr("wo", [8, 128, KOC, 256])
    g_d = dr("g", [128, NK])
    wgu_d = dr("wgu", [E, NFC, 128, 2, NK, 128])
    wd_d = dr("wd", [E, NS, 8, 128, SGC, 256])
    cst_d = dr("cst", [128, 128])
    if E > 1:
        rt_d = dr("rt", [128, NK, E])
    out_d = nc.dram_tensor("out", [128, NK, T], F32, kind="ExternalOutput").ap()

    with contextlib.ExitStack() as st:
        c = Ctx(nc, st)
        PS = [Tile(c.psum([128, 512], F32), f"bank{i}") for i in range(8)]
        h1 = [Tile(None, f"h1_{k}") for k in range(NK)]
        h1_sb = c.sbuf([128, NK, T], F32, "h1")
        for k in range(NK):
            h1[k].ap = h1_sb[:, k, :]
        g_t = Tile(c.sbuf([128, NK], F32, "g"), "g")
        ones_bf = Tile(c.sbuf([128, 128], BF16, "ones"), "ones")
        ident = Tile(c.sbuf([128, 128], F32, "ident"), "ident")
        rstd = Tile(c.sbuf([128, T], F32, "rstd"), "rstd")
        out_t = Tile(out_d, "out")
        st.enter_context(nc.Block())

        c.op(c.dve, lambda e: e.memset(ones_bf[:, :], 1.0), writes=[ones_bf])
        c.dma(c.sp, g_t[:, :], g_d, dst=g_t)
        c.dma(c.sp, ident[:, :], cst_d, dst=ident)
        for k in range(NK):
            c.dma(c.sp, h1[k].ap, hT_d[:, k, :], dst=h1[k])

        with contextlib.ExitStack() as st1:
            c.stack = st1
            oT = Tile(c.sbuf([128, KOC, T], BF16, "oT"), "oT")
            wo = [Tile(c.sbuf([128, KOC, 256], BF16, f"wo{i}"), f"wo{i}") for i in range(2)]
            for q in range(4):
                k0, k1 = q * KOC // 4, (q + 1) * KOC // 4
                c.dma(c.sp, oT[:, k0:k1, :], oT_d[:, k0:k1, :], dst=oT)
            last = None
            for o in range(8):
                w = wo[o % 2]
                c.dma(c.pool, w[:, :, :], wo_d[o], dst=w, max_dma_last_dim=8192)
                for k in range(KOC):
                    for f in range(2):
                        for th in range(2):
                            bank = PS[(o % 2) * 4 + f * 2 + th]
                            c.op(c.pe, lambda e, bank=bank, k=k, f=f, th=th, w=w: e.matmul(
                                bank[:, :], lhsT=w[:, k, f * 128:(f + 1) * 128],
                                rhs=oT[:, k, th * 512:(th + 1) * 512],
                                start=(k == 0), stop=(k == KOC - 1)),
                                reads=[w, oT], writes=[bank], signal=(k == KOC - 1))
                for f in range(2):
                    for th in range(2):
                        bank = PS[(o % 2) * 4 + f * 2 + th]
                        hk = h1[o * 2 + f]
                        last = c.op(c.dve, lambda e, bank=bank, hk=hk, th=th: e.tensor_tensor(
                            out=hk.ap[:, th * 512:(th + 1) * 512], in0=hk.ap[:, th * 512:(th + 1) * 512],
                            in1=bank[:, :], op=ALU.add), reads=[bank, hk], writes=[hk])
            c.stack = st
        barrier = last

        def fresh(t):
            t.last_write = barrier
            return t

        xn_sb = c.sbuf([128, NK, T], BF16, "xn")
        xn = Tile(xn_sb, "xn")
        fresh(xn)
        sq = [fresh(Tile(c.sbuf([128, T], BF16, f"sq{i}"), f"sq{i}")) for i in range(2)]
        for k in range(NK):
            s = sq[k % 2]
            c.op(c.act, lambda e, s=s, k=k: e.activation(out=s[:, :], in_=h1[k].ap, func=AF.Square),
                 reads=[h1[k]], writes=[s])
            for th in range(2):
                c.op(c.pe, lambda e, s=s, k=k, th=th: e.matmul(
                    PS[th][:, :], lhsT=ones_bf[:, :], rhs=s[:, th * 512:(th + 1) * 512],
                    start=(k == 0), stop=(k == NK - 1)),
                    reads=[ones_bf, s], writes=[PS[th]], signal=(k == NK - 1 or True))
        for th in range(2):
            sl = slice(th * 512, (th + 1) * 512)
            c.op(c.dve, lambda e, th=th, sl=sl: e.tensor_scalar(
                out=rstd[:, sl], in0=PS[th][:, :], scalar1=1.0 / D, scalar2=EPS,
                op0=ALU.mult, op1=ALU.add), reads=[PS[th]], writes=[rstd])
        c.op(c.act, lambda e: e.activation(out=rstd[:, :], in_=rstd[:, :], func=AF.Sqrt),
             reads=[rstd], writes=[rstd])
        c.op(c.dve, lambda e: e.reciprocal(out=rstd[:, :], in_=rstd[:, :]), reads=[rstd], writes=[rstd])
        for k in range(NK):
            c.op(c.dve, lambda e, k=k: e.scalar_tensor_tensor(
                out=xn_sb[:, k, :], in0=h1[k].ap, scalar=g_t[:, k:k + 1], in1=rstd[:, :],
                op0=ALU.mult, op1=ALU.mult), reads=[h1[k], g_t, rstd], writes=[xn])

        if E > 1:
            gb_sb = c.sbuf([128, E, T], BF16, "gb")
            gb = fresh(Tile(gb_sb, "gb"))
            rt = fresh(Tile(c.sbuf([128, NK, E], F32, "rt"), "rt"))
            c128 = Tile(c.sbuf([128, 2], F32, "c128"), "c128")
            ident_bf = Tile(c.sbuf([128, 128], BF16, "identbf"), "identbf")
            sm = fresh(Tile(c.sbuf([128, 8, 64], F32, "sm"), "sm"))
            diag = [fresh(Tile(c.sbuf([128, E, 128], BF16, f"diag{i}"), f"diag{i}")) for i in range(2)]
            c.dma(c.sp, rt[:, :, :], rt_d, dst=rt)
            c.op(c.dve, lambda e: e.memset(c128[:, :], 1.0 / 128), writes=[c128])
            c.op(c.dve, lambda e: e.tensor_copy(out=ident_bf[:, :], in_=ident[:, :]), reads=[ident], writes=[ident_bf])
            for k in range(NK):
                c.op(c.dve, lambda e, k=k: e.tensor_scalar(
                    out=rt[:, k, :], in0=rt[:, k, :], scalar1=g_t[:, k:k + 1], scalar2=None,
                    op0=ALU.mult), reads=[rt, g_t], writes=[rt])
            for tt in range(T // 128):
                tsl = slice(tt * 128, (tt + 1) * 128)
                pl = PS[2 + (tt % 2) * 3]
                pb0, pb1 = PS[3 + (tt % 2) * 3], PS[4 + (tt % 2) * 3]
                for k in range(NK):
                    c.op(c.pe, lambda e, k=k, pl=pl, tsl=tsl: e.matmul(
                        pl[:, 0:E], lhsT=h1_sb[:, k, tsl], rhs=rt[:, k, :],
                        start=(k == 0), stop=(k == NK - 1)),
                        reads=[h1[k], rt], writes=[pl], signal=(k == NK - 1))
                c.op(c.pe, lambda e, pl=pl, tsl=tsl: e.matmul(
                    pl[:, 16:18], lhsT=rstd[:, tsl], rhs=c128[:, :], start=True, stop=True),
                    reads=[rstd, c128], writes=[pl])
                S = lambda i, n=E: sm[:, tt, i * 8:i * 8 + n]
                rs = sm[:, tt, 56:57]
                c.op(c.dve, lambda e: e.tensor_copy(out=sm[:, tt, 56:58], in_=pl[:, 16:18]), reads=[pl], writes=[sm])
                c.op(c.dve, lambda e: e.tensor_copy(out=S(0), in_=pl[:, 0:E]), reads=[pl], writes=[sm])
                c.op(c.dve, lambda e: e.tensor_reduce(out=S(1, 1), in_=S(0), axis=AX.X, op=ALU.max), reads=[sm], writes=[sm])
                c.op(c.dve, lambda e: e.tensor_scalar(out=S(2), in0=S(0), scalar1=S(1, 1), scalar2=-1e30,
                                                      op0=ALU.is_equal, op1=ALU.mult), reads=[sm], writes=[sm])
                c.op(c.dve, lambda e: e.tensor_tensor(out=S(2), in0=S(2), in1=S(0), op=ALU.add), reads=[sm], writes=[sm])
                c.op(c.dve, lambda e: e.tensor_reduce(out=S(3, 1), in_=S(2), axis=AX.X, op=ALU.max), reads=[sm], writes=[sm])
                c.op(c.dve, lambda e: e.tensor_scalar(out=S(4), in0=S(0), scalar1=S(3, 1), scalar2=None,
                                                      op0=ALU.is_ge), reads=[sm], writes=[sm])
                c.op(c.dve, lambda e: e.tensor_scalar(out=S(5), in0=S(0), scalar1=S(1, 1), scalar2=rs,
                                                      op0=ALU.subtract, op1=ALU.mult), reads=[sm], writes=[sm])
                c.op(c.act, lambda e: e.activation(out=S(5), in_=S(5), func=AF.Exp), reads=[sm], writes=[sm])
                c.op(c.dve, lambda e: e.tensor_tensor(out=S(5), in0=S(5), in1=S(4), op=ALU.mult), reads=[sm], writes=[sm])
                c.op(c.dve, lambda e: e.tensor_reduce(out=S(6, 1), in_=S(5), axis=AX.X, op=ALU.add), reads=[sm], writes=[sm])
                c.op(c.dve, lambda e: e.reciprocal(out=S(6, 1), in_=S(6, 1)), reads=[sm], writes=[sm])
                c.op(c.dve, lambda e: e.tensor_scalar(out=S(5), in0=S(5), scalar1=S(6, 1), scalar2=None,
                                                      op0=ALU.mult), reads=[sm], writes=[sm])
                dg = diag[tt % 2]
                for ex in range(E):
                    c.op(c.dve, lambda e, ex=ex, dg=dg: e.tensor_scalar(
                        out=dg[:, ex, :], in0=ident_bf[:, :], scalar1=sm[:, tt, 40 + ex:41 + ex], scalar2=None,
                        op0=ALU.mult), reads=[sm, ident_bf], writes=[dg])
                for half, pb in ((0, pb0), (1, pb1)):
                    c.op(c.pe, lambda e, half=half, pb=pb, dg=dg: e.matmul(
                        pb[:, :], lhsT=ones_bf[:, :], rhs=dg[:, half * 4:(half + 1) * 4, :],
                        start=True, stop=True), reads=[ones_bf, dg], writes=[pb])
                    c.op(c.act, lambda e, half=half, pb=pb, tsl=tsl: e.activation(
                        out=gb_sb[:, half * 4:(half + 1) * 4, tsl],
                        in_=pb[:, :].rearrange("p (a b) -> p a b", a=4), func=AF.Copy),
                        reads=[pb], writes=[gb])

        act_sb = c.sbuf([128, SGC, T], BF16, "act")
        actT = fresh(Tile(act_sb, "act"))
        gu = [fresh(Tile(c.sbuf([128, 2, NK, 128], BF16, f"gu{i}"), f"gu{i}")) for i in range(2)]
        wd = [fresh(Tile(c.sbuf([128, SGC, 256], BF16, f"wd{i}"), f"wd{i}")) for i in range(2)]
        stile = [fresh(Tile(c.sbuf([128, 512], F32, f"s{i}"), f"s{i}")) for i in range(2)]
        utile = [fresh(Tile(c.sbuf([128, 512], F32, f"u{i}"), f"u{i}")) for i in range(2)]
        gi = 0
        di = 0
        for ex in range(E):
            for s in range(NS):
                for j in range(SGC):
                    fc = s * SGC + j
                    w = gu[gi % 2]
                    pb = (gi % 2) * 4
                    gi += 1
                    c.dma(c.pool, w[:, :, :, :], wgu_d[ex, fc], dst=w, max_dma_last_dim=8192)
                    for which in range(2):
                        for k in range(NK):
                            for th in range(2):
                                bank = PS[pb + which * 2 + th]
                                c.op(c.pe, lambda e, bank=bank, w=w, which=which, k=k, th=th: e.matmul(
                                    bank[:, :], lhsT=w[:, which, k, :], rhs=xn_sb[:, k, th * 512:(th + 1) * 512],
                                    start=(k == 0), stop=(k == NK - 1)),
                                    reads=[w, xn], writes=[bank], signal=(k == NK - 1))
                    for th in range(2):
                        sl = slice(th * 512, (th + 1) * 512)
                        sb_, ub_ = stile[th], utile[th]
                        c.op(c.act, lambda e, sb_=sb_, th=th: e.activation(
                            out=sb_[:, :], in_=PS[pb + th][:, :], func=AF.Silu),
                            reads=[PS[pb + th]], writes=[sb_])
                        if E > 1:
                            c.op(c.dve, lambda e, ub_=ub_, th=th, sl=sl: e.tensor_tensor(
                                out=ub_[:, :], in0=PS[pb + 2 + th][:, :], in1=gb_sb[:, ex, sl], op=ALU.mult),
                                reads=[PS[pb + 2 + th], gb], writes=[ub_])
                            c.op(c.dve, lambda e, ub_=ub_, sb_=sb_, sl=sl: e.tensor_tensor(
                                out=act_sb[:, j, sl], in0=sb_[:, :], in1=ub_[:, :], op=ALU.mult),
                                reads=[sb_, ub_], writes=[actT])
                        else:
                            c.op(c.dve, lambda e, sb_=sb_, th=th, sl=sl: e.tensor_tensor(
                                out=act_sb[:, j, sl], in0=sb_[:, :], in1=PS[pb + 2 + th][:, :], op=ALU.mult),
                                reads=[sb_, PS[pb + 2 + th]], writes=[actT])
                for o in range(8):
                    w = wd[di % 2]
                    pb = (di % 2) * 4
                    di += 1
                    c.dma(c.pool, w[:, :, :], wd_d[ex, s, o], dst=w, max_dma_last_dim=8192)
                    for k in range(SGC):
                        for f in range(2):
                            for th in range(2):
                                bank = PS[pb + f * 2 + th]
                                c.op(c.pe, lambda e, bank=bank, w=w, k=k, f=f, th=th: e.matmul(
                                    bank[:, :], lhsT=w[:, k, f * 128:(f + 1) * 128],
                                    rhs=act_sb[:, k, th * 512:(th + 1) * 512],
                                    start=(k == 0), stop=(k == SGC - 1)),
                                    reads=[w, actT], writes=[bank], signal=(k == SGC - 1))
                    for f in range(2):
                        for th in range(2):
                            bank = PS[pb + f * 2 + th]
                            hk = h1[o * 2 + f]
                            sl = slice(th * 512, (th + 1) * 512)
                            c.op(c.dve, lambda e, bank=bank, hk=hk, sl=sl: e.tensor_tensor(
                                out=hk.ap[:, sl], in0=hk.ap[:, sl], in1=bank[:, :], op=ALU.add),
                                reads=[bank, hk], writes=[hk])
        for k in range(NK):
            c.dma(c.sp, out_d[:, k, :], h1[k].ap, dst=out_t, src=h1[k])
        c.finish([out_t])
    return nc


def lay_feat_major(a):
    t, f = a.shape
    return np.ascontiguousarray(a.T.reshape(f // 128, 128, t).transpose(1, 0, 2))


def unlay_feat_major(a):
    p, fc, t = a.shape
    return np.ascontiguousarray(a.transpose(1, 0, 2).reshape(fc * 128, t).T)


def lay_w_cols(w, cw):
    k, n = w.shape
    return np.ascontiguousarray(w.reshape(k // 128, 128, n // cw, cw).transpose(2, 1, 0, 3))


def lay_vec(g):
    return np.ascontiguousarray(g.reshape(-1, 128).T)


def ffn_weights(w_out, g, w_gate, w_up, w_down, router=None):
    E, _, FH = w_gate.shape
    NS = FH // (SGC * 128)
    wgu = np.stack([np.stack([lay_w_cols(w_gate[e], 128), lay_w_cols(w_up[e], 128)], axis=2)
                    for e in range(E)])
    wd = np.stack([np.stack([lay_w_cols(w_down[e][s * SGC * 128:(s + 1) * SGC * 128], 256)
                             for s in range(NS)]) for e in range(E)])
    d = {"wo": lay_w_cols(w_out, 256), "g": lay_vec(g), "wgu": wgu, "wd": wd,
         "cst": np.eye(128, dtype=np.float32)}
    if router is not None:
        d["rt"] = np.ascontiguousarray(router.reshape(NK, 128, -1).transpose(1, 0, 2))
    return d


NH = 8


def barrier_all(c):
    return {id(e.sem): (e.sem, e.count) for e in (c.pe, c.act, c.dve, c.pool) if e.count > 0}


def fresh(t, bar):
    t.readers = dict(bar)
    return t


def rmsnorm_T(c, PS, hT_d, g_t, ones_bf, hn_sb, hn, ntok):
    with contextlib.ExitStack() as st1:
        root = c.stack
        c.stack = st1
        hb = [Tile(c.sbuf([128, NK, 512], F32, f"hb{i}"), f"hb{i}") for i in range(2)]
        sq = [Tile(c.sbuf([128, 512], BF16, f"sq{i}"), f"sq{i}") for i in range(2)]
        rstd = Tile(c.sbuf([128, 512], F32, "rstd"), "rstd")
        for tp in range(ntok // 512):
            tsl = slice(tp * 512, (tp + 1) * 512)
            hbt = hb[tp % 2]
            for q in range(4):
                c.dma(c.sp, hbt[:, q * 4:(q + 1) * 4, :], hT_d[:, q * 4:(q + 1) * 4, tsl], dst=hbt)
            bank = PS[tp % 2]
            for k in range(NK):
                s = sq[k % 2]
                c.op(c.act, lambda e: e.activation(out=s[:, :], in_=hbt[:, k, :], func=AF.Square),
                     reads=[hbt], writes=[s])
                c.op(c.pe, lambda e: e.matmul(bank[:, :], lhsT=ones_bf[:, :], rhs=s[:, :],
                                              start=(k == 0), stop=(k == NK - 1)),
                     reads=[ones_bf, s], writes=[bank])
            c.op(c.dve, lambda e: e.tensor_scalar(out=rstd[:, :], in0=bank[:, :], scalar1=1.0 / D, scalar2=EPS,
                                                  op0=ALU.mult, op1=ALU.add), reads=[bank], writes=[rstd])
            c.op(c.act, lambda e: e.activation(out=rstd[:, :], in_=rstd[:, :], func=AF.Sqrt),
                 reads=[rstd], writes=[rstd])
            c.op(c.dve, lambda e: e.reciprocal(out=rstd[:, :], in_=rstd[:, :]), reads=[rstd], writes=[rstd])
            for k in range(NK):
                c.op(c.dve, lambda e: e.scalar_tensor_tensor(
                    out=hn_sb[:, k, tsl], in0=hbt[:, k, :], scalar=g_t[:, k:k + 1], in1=rstd[:, :],
                    op0=ALU.mult, op1=ALU.mult), reads=[hbt, g_t, rstd], writes=[hn])
        c.stack = root
    return barrier_all(c)


def build_fox():
    nc = bass.Bass("TRN2", target_bir_lowering=False)
    dr = lambda n, s, dt=F32: nc.dram_tensor(n, list(s), dt, kind="ExternalInput").ap()
    hT_d = dr("hT", [128, NK, S])
    g_d = dr("g", [128, NK])
    wqko_d = dr("wqko", [NH, 128, 3, NK, 128])
    wv_d = dr("wv", [2, 128, NK, 512])
    wf_d = dr("wf", [128, NK, NH])
    bf_d = dr("bf", [128, NH])
    gn_d = dr("gn", [128, 2])
    cst_d = dr("cst", [128, 256])
    out_d = nc.dram_tensor("out", [128, NH, S], BF16, kind="ExternalOutput").ap()

    with contextlib.ExitStack() as st:
        c = Ctx(nc, st)
        PS = [Tile(c.psum([128, 512], F32), f"bank{i}") for i in range(8)]
        g_t = Tile(c.sbuf([128, NK], F32, "g"), "g")
        ones_bf = Tile(c.sbuf([128, 128], BF16, "ones"), "ones")
        ones_f = Tile(c.sbuf([128, 128], F32, "onesf"), "onesf")
        cst = Tile(c.sbuf([128, 256], F32, "cst"), "cst")
        tri_bf = Tile(c.sbuf([128, 128], BF16, "tri"), "tri")
        gn = Tile(c.sbuf([128, 2], F32, "gn"), "gn")
        bfb = Tile(c.sbuf([128, NH], F32, "bfb"), "bfb")
        hn_sb = c.sbuf([128, NK, S], BF16, "hn")
        hn = Tile(hn_sb, "hn")
        out_t = Tile(out_d, "out")
        st.enter_context(nc.Block())
        c.op(c.dve, lambda e: e.memset(ones_bf[:, :], 1.0), writes=[ones_bf])
        c.op(c.dve, lambda e: e.memset(ones_f[:, :], 1.0), writes=[ones_f])
        c.dma(c.sp, g_t[:, :], g_d, dst=g_t)
        c.dma(c.sp, cst[:, :], cst_d, dst=cst)
        c.dma(c.sp, gn[:, :], gn_d, dst=gn)
        c.dma(c.sp, bfb[:, :], bf_d, dst=bfb)
        c.op(c.dve, lambda e: e.tensor_copy(out=tri_bf[:, :], in_=cst[:, 128:256]), reads=[cst], writes=[tri_bf])
        c.op(c.dve, lambda e: e.tensor_scalar(out=gn[:, 0:1], in0=gn[:, 0:1], scalar1=128 ** -0.5, scalar2=None,
                                              op0=ALU.mult), reads=[gn], writes=[gn])

        bar = rmsnorm_T(c, PS, hT_d, g_t, ones_bf, hn_sb, hn, S)

        v_sb = c.sbuf([128, 16, NH * 128], BF16, "v")
        v = fresh(Tile(v_sb, "v"), bar)
        logf = fresh(Tile(c.sbuf([128, 16, NH], F32, "logf"), "logf"), bar)
        Rb = fresh(Tile(c.sbuf([128, 17, NH], F32, "Rb"), "Rb"), bar)
        Wb = fresh(Tile(c.sbuf([128, 16, NH], F32, "Wb"), "Wb"), bar)
        bias = fresh(Tile(c.sbuf([128, 136, NH], F32, "bias"), "bias"), bar)
        wf = fresh(Tile(c.sbuf([128, NK, NH], BF16, "wf"), "wf"), bar)
        with contextlib.ExitStack() as st2:
            c.stack = st2
            wv = [fresh(Tile(c.sbuf([128, NK, 512], BF16, f"wv{i}"), f"wv{i}"), bar) for i in range(2)]
            c.dma(c.pool, wf[:, :, :], wf_d, dst=wf)
            for cg in range(2):
                c.dma(c.pool, wv[cg][:, :, :], wv_d[cg], dst=wv[cg], max_dma_last_dim=8192)
            for tb in range(16):
                tsl = slice(tb * 128, (tb + 1) * 128)
                for cg in range(2):
                    bank = PS[(tb * 2 + cg) % 4]
                    for k in range(NK):
                        c.op(c.pe, lambda e: e.matmul(bank[:, :], lhsT=hn_sb[:, k, tsl], rhs=wv[cg][:, k, :],
                                                      start=(k == 0), stop=(k == NK - 1)),
                             reads=[hn, wv[cg]], writes=[bank], signal=(k == NK - 1))
                    eng = c.act if cg == 0 else c.dve
                    if cg == 0:
                        c.op(c.act, lambda e: e.activation(out=v_sb[:, tb, 0:512], in_=bank[:, :], func=AF.Copy),
                             reads=[bank], writes=[v])
                    else:
                        c.op(c.dve, lambda e: e.tensor_copy(out=v_sb[:, tb, 512:1024], in_=bank[:, :]),
                             reads=[bank], writes=[v])
                fb = PS[4 + tb % 2]
                for k in range(NK):
                    c.op(c.pe, lambda e: e.matmul(fb[:, 0:NH], lhsT=hn_sb[:, k, tsl], rhs=wf[:, k, :],
                                                  start=(k == 0), stop=(k == NK - 1)),
                         reads=[hn, wf], writes=[fb], signal=(k == NK - 1))
                c.op(c.dve, lambda e: e.tensor_tensor(out=logf[:, tb, :], in0=fb[:, 0:NH], in1=bfb[:, :], op=ALU.add),
                     reads=[fb, bfb], writes=[logf])
            c.stack = st
        c.op(c.act, lambda e: e.activation(out=logf[:, :, :], in_=logf[:, :, :], func=AF.Exp, scale=-1.0),
             reads=[logf], writes=[logf])
        c.op(c.dve, lambda e: e.tensor_scalar(out=logf[:, :, :], in0=logf[:, :, :], scalar1=1.0, scalar2=None,
                                              op0=ALU.add), reads=[logf], writes=[logf])
        c.op(c.act, lambda e: e.activation(out=logf[:, :, :], in_=logf[:, :, :], func=AF.Ln),
             reads=[logf], writes=[logf])
        c.op(c.dve, lambda e: e.tensor_scalar(out=logf[:, :, :], in0=logf[:, :, :], scalar1=-1.0, scalar2=None,
                                              op0=ALU.mult), reads=[logf], writes=[logf])
        c.op(c.dve, lambda e: e.memset(Rb[:, 0, :], 0.0), writes=[Rb])
        for j in range(16):
            pb = PS[6 + j % 2]
            c.op(c.pe, lambda e: e.matmul(pb[:, 0:NH], lhsT=ones_f[:, :], rhs=logf[:, j, :], start=True, stop=True),
                 reads=[ones_f, logf], writes=[pb])
            c.op(c.pe, lambda e: e.matmul(pb[:, 16:16 + NH], lhsT=cst[:, 0:128], rhs=logf[:, j, :], start=True, stop=True),
                 reads=[cst, logf], writes=[pb])
            c.op(c.dve, lambda e: e.tensor_tensor(out=Rb[:, j + 1, :], in0=Rb[:, j, :], in1=pb[:, 0:NH], op=ALU.add),
                 reads=[pb, Rb], writes=[Rb])
            c.op(c.dve, lambda e: e.tensor_copy(out=Wb[:, j, :], in_=pb[:, 16:16 + NH]), reads=[pb], writes=[Wb])
        c.op(c.dve, lambda e: e.tensor_tensor(out=Wb[:, :, :], in0=Wb[:, :, :], in1=Rb[:, 0:16, :], op=ALU.add),
             reads=[Wb, Rb], writes=[Wb])
        bidx = lambda qb, j: qb * (qb + 1) // 2 + j
        for qb in range(16):
            for j in range(qb + 1):
                c.op(c.dve, lambda e: e.tensor_tensor(out=bias[:, bidx(qb, j), :], in0=Rb[:, qb, :], in1=Wb[:, j, :],
                                                      op=ALU.subtract), reads=[Rb, Wb], writes=[bias])
        bar = barrier_all(c)

        wq = [fresh(Tile(c.sbuf([128, 3, NK, 128], BF16, f"wq{i}"), f"wq{i}"), bar) for i in range(2)]
        qT = [fresh(Tile(c.sbuf([128, S], BF16, f"qT{i}"), f"qT{i}"), bar) for i in range(2)]
        kT = [fresh(Tile(c.sbuf([128, S], BF16, f"kT{i}"), f"kT{i}"), bar) for i in range(2)]
        sqb = [fresh(Tile(c.sbuf([128, 512], BF16, f"sqb{i}"), f"sqb{i}"), bar) for i in range(2)]
        rsd = [fresh(Tile(c.sbuf([128, 512], F32, f"rsd{i}"), f"rsd{i}"), bar) for i in range(2)]
        Pt = [fresh(Tile(c.sbuf([128, 512], BF16, f"P{i}"), f"P{i}"), bar) for i in range(3)]
        rinv = fresh(Tile(c.sbuf([128, 512], F32, "rinv"), "rinv"), bar)
        onrm = fresh(Tile(c.sbuf([128, 512], F32, "onrm"), "onrm"), bar)
        sig = fresh(Tile(c.sbuf([128, 512], F32, "sig"), "sig"), bar)
        ob = [fresh(Tile(c.sbuf([128, 512], BF16, f"ob{i}"), f"ob{i}"), bar) for i in range(2)]
        pcount = 0
        ocount = 0
        for h in range(NH):
            w = wq[h % 2]
            c.dma(c.pool, w[:, :, :, :], wqko_d[h], dst=w, max_dma_last_dim=8192)
            for which, dstT in ((0, qT[h % 2]), (1, kT[h % 2])):
                for tp in range(4):
                    tsl = slice(tp * 512, (tp + 1) * 512)
                    bank = PS[tp % 2]
                    for k in range(NK):
                        c.op(c.pe, lambda e: e.matmul(bank[:, :], lhsT=w[:, which, k, :], rhs=hn_sb[:, k, tsl],
                                                      start=(k == 0), stop=(k == NK - 1)),
                             reads=[w, hn], writes=[bank], signal=(k == NK - 1))
                    sq_ = sqb[tp % 2]
                    rs_ = rsd[tp % 2]
                    sbank = PS[2 + tp % 2]
                    c.op(c.act, lambda e: e.activation(out=sq_[:, :], in_=bank[:, :], func=AF.Square),
                         reads=[bank], writes=[sq_])
                    c.op(c.pe, lambda e: e.matmul(sbank[:, :], lhsT=ones_bf[:, :], rhs=sq_[:, :], start=True, stop=True),
                         reads=[ones_bf, sq_], writes=[sbank])
                    c.op(c.dve, lambda e: e.tensor_scalar(out=rs_[:, :], in0=sbank[:, :], scalar1=1.0 / 128, scalar2=EPS,
                                                          op0=ALU.mult, op1=ALU.add), reads=[sbank], writes=[rs_])
                    c.op(c.act, lambda e: e.activation(out=rs_[:, :], in_=rs_[:, :], func=AF.Sqrt),
                         reads=[rs_], writes=[rs_])
                    c.op(c.dve, lambda e: e.reciprocal(out=rs_[:, :], in_=rs_[:, :]), reads=[rs_], writes=[rs_])
                    c.op(c.dve, lambda e: e.scalar_tensor_tensor(
                        out=dstT[:, tsl], in0=bank[:, :], scalar=gn[:, which:which + 1], in1=rs_[:, :],
                        op0=ALU.mult, op1=ALU.mult), reads=[bank, gn, rs_], writes=[dstT])
            q_, k_ = qT[h % 2], kT[h % 2]
            for g in range(4):
                obank, rbank, gbank = PS[4], PS[5], PS[2 + g % 2]
                nj = 4 * g + 4
                for j in range(nj):
                    jj = j - 4 * g
                    q0 = max(jj, 0) * 128
                    sbank = PS[6 + pcount % 2]
                    P_ = Pt[pcount % 3]
                    pcount += 1
                    c.op(c.pe, lambda e: e.matmul(sbank[:, q0:512], lhsT=k_[:, j * 128:(j + 1) * 128],
                                                  rhs=q_[:, g * 512 + q0:(g + 1) * 512], start=True, stop=True),
                         reads=[k_, q_], writes=[sbank])
                    for qq in range(max(jj, 0), 4):
                        qb = 4 * g + qq
                        c.op(c.act, lambda e: e.activation(
                            out=P_[:, qq * 128:(qq + 1) * 128], in_=sbank[:, qq * 128:(qq + 1) * 128], func=AF.Exp,
                            bias=bias[:, bidx(qb, j), h:h + 1], scale=1.0),
                            reads=[sbank, bias], writes=[P_], signal=(qq == 3))
                    if jj >= 0:
                        c.op(c.dve, lambda e: e.tensor_tensor(out=P_[:, q0:q0 + 128], in0=P_[:, q0:q0 + 128],
                                                              in1=tri_bf[:, :], op=ALU.mult),
                             reads=[P_, tri_bf], writes=[P_])
                    c.op(c.pe, lambda e: e.matmul(obank[:, q0:512], lhsT=v_sb[:, j, h * 128:(h + 1) * 128],
                                                  rhs=P_[:, q0:512], start=(j == 0), stop=(j == nj - 1)),
                         reads=[v, P_], writes=[obank], signal=False)
                    c.op(c.pe, lambda e: e.matmul(rbank[:, q0:512], lhsT=ones_bf[:, :],
                                                  rhs=P_[:, q0:512], start=(j == 0), stop=(j == nj - 1)),
                         reads=[ones_bf, P_], writes=[rbank, obank], signal=True)
                for k in range(NK):
                    c.op(c.pe, lambda e: e.matmul(gbank[:, :], lhsT=w[:, 2, k, :], rhs=hn_sb[:, k, g * 512:(g + 1) * 512],
                                                  start=(k == 0), stop=(k == NK - 1)),
                         reads=[w, hn], writes=[gbank], signal=(k == NK - 1))
                c.op(c.dve, lambda e: e.reciprocal(out=rinv[:, :], in_=rbank[:, :]), reads=[rbank], writes=[rinv])
                c.op(c.dve, lambda e: e.tensor_tensor(out=onrm[:, :], in0=obank[:, :], in1=rinv[:, :], op=ALU.mult),
                     reads=[obank, rinv], writes=[onrm])
                c.op(c.act, lambda e: e.activation(out=sig[:, :], in_=gbank[:, :], func=AF.Sigmoid),
                     reads=[gbank], writes=[sig])
                o_ = ob[ocount % 2]
                ocount += 1
                c.op(c.dve, lambda e: e.tensor_tensor(out=o_[:, :], in0=onrm[:, :], in1=sig[:, :], op=ALU.mult),
                     reads=[onrm, sig], writes=[o_])
                c.dma(c.sp, out_d[:, h, g * 512:(g + 1) * 512], o_[:, :], dst=out_t, src=o_)
        c.finish([out_t])
    return nc


def lay_hT(hb):
    return np.ascontiguousarray(hb.T.reshape(NK, 128, -1).transpose(1, 0, 2))


def lay_wcols(w):
    return np.ascontiguousarray(w.reshape(NK, 128, -1).transpose(1, 0, 2))


def fox_consts():
    U = np.triu(np.ones((128, 128), np.float32))
    return np.ascontiguousarray(np.concatenate([U, U], axis=1))


def fox_weights(w_in, b_f, q_gain, k_gain, g_mix, hh):
    W = 2048
    cols = slice(hh * 1024, (hh + 1) * 1024)
    wq = lay_wcols(w_in[:, 0 * W:1 * W][:, cols]).reshape(128, NK, NH, 128)
    wk = lay_wcols(w_in[:, 1 * W:2 * W][:, cols]).reshape(128, NK, NH, 128)
    wv = lay_wcols(w_in[:, 2 * W:3 * W][:, cols]).reshape(128, NK, 2, 512)
    wo = lay_wcols(w_in[:, 3 * W:4 * W][:, cols]).reshape(128, NK, NH, 128)
    wf = lay_wcols(w_in[:, 4 * W + hh * NH:4 * W + (hh + 1) * NH])
    wqko = np.stack([wq, wk, wo], axis=0)
    wqko = np.ascontiguousarray(wqko.transpose(3, 1, 0, 2, 4))
    return {
        "g": np.ascontiguousarray(g_mix.reshape(NK, 128).T),
        "wqko": wqko,
        "wv": np.ascontiguousarray(wv.transpose(2, 0, 1, 3)),
        "wf": wf,
        "bf": np.ascontiguousarray(np.broadcast_to(b_f[hh * NH:(hh + 1) * NH][None, :], (128, NH))),
        "gn": np.ascontiguousarray(np.stack([q_gain, k_gain], axis=1)),
        "cst": fox_consts(),
    }


NKH = 8
NVH = 16


def build_gdn():
    nc = bass.Bass("TRN2", target_bir_lowering=False)
    dr = lambda n, s, dt=F32: nc.dram_tensor(n, list(s), dt, kind="ExternalInput").ap()
    hT_d = dr("hT", [128, NK, S])
    g_d = dr("g", [128, NK])
    wkh_d = dr("wkh", [NKH, 128, NK, 768])
    wab_d = dr("wab", [128, NK, 32])
    cw_d = dr("cw", [128, NKH, 4, 4])
    pv_d = dr("pv", [16, 2])
    ogr_d = dr("ogr", [128, 256])
    cst_d = dr("cst", [128, 5, 128])
    out_dt = nc.dram_tensor("out", [S, NVH, 128], BF16, kind="ExternalOutput")
    out_d = out_dt.ap().rearrange("(c p) v e -> p c v e", p=128)

    with contextlib.ExitStack() as st:
        c = Ctx(nc, st)
        PS = [Tile(c.psum([128, 512], F32), f"bank{i}") for i in range(8)]
        g_t = Tile(c.sbuf([128, NK], F32, "g"), "g")
        ones_bf = Tile(c.sbuf([128, 128], BF16, "ones"), "ones")
        cst = Tile(c.sbuf([128, 5, 128], F32, "cst"), "cst")
        ident_bf = Tile(c.sbuf([128, 128], BF16, "identbf"), "identbf")
        ogr = Tile(c.sbuf([128, 256], F32, "ogr"), "ogr")
        cw = Tile(c.sbuf([128, NKH, 4, 4], F32, "cw"), "cw")
        pv = Tile(c.sbuf([16, 2], F32, "pv"), "pv")
        nA = Tile(c.sbuf([16, 1], F32, "nA"), "nA")
        hn_sb = c.sbuf([128, NK, S], BF16, "hn")
        hn = Tile(hn_sb, "hn")
        gcT = Tile(c.sbuf([16, S], F32, "gcT"), "gcT")
        bT = Tile(c.sbuf([16, S], F32, "bT"), "bT")
        gc_col = Tile(c.sbuf([128, 16, 16], F32, "gccol"), "gccol")
        b_col = Tile(c.sbuf([128, 16, 16], F32, "bcol"), "bcol")
        negb_col = Tile(c.sbuf([128, 16, 16], F32, "negbcol"), "negbcol")
        bec = Tile(c.sbuf([128, 16, 16], F32, "bec"), "bec")
        dcol = Tile(c.sbuf([128, 16, 16], F32, "dcol"), "dcol")
        out_t = Tile(out_d, "out")
        st.enter_context(nc.Block())
        c.op(c.dve, lambda e: e.memset(ones_bf[:, :], 1.0), writes=[ones_bf])
        c.dma(c.sp, g_t[:, :], g_d, dst=g_t)
        c.dma(c.sp, cst[:, :, :], cst_d, dst=cst)
        c.dma(c.sp, ogr[:, :], ogr_d, dst=ogr)
        c.dma(c.sp, cw[:, :, :, :], cw_d, dst=cw)
        c.dma(c.sp, pv[:, :], pv_d, dst=pv)
        c.op(c.dve, lambda e: e.tensor_copy(out=ident_bf[:, :], in_=cst[:, 0, :]), reads=[cst], writes=[ident_bf])
        c.op(c.act, lambda e: e.activation(out=nA[:, :], in_=pv[:, 0:1], func=AF.Exp), reads=[pv], writes=[nA])
        c.op(c.dve, lambda e: e.tensor_scalar(out=nA[:, :], in0=nA[:, :], scalar1=-1.0, scalar2=None, op0=ALU.mult),
             reads=[nA], writes=[nA])

        bar = rmsnorm_T(c, PS, hT_d, g_t, ones_bf, hn_sb, hn, S)

        with contextlib.ExitStack() as st2:
            c.stack = st2
            wab = fresh(Tile(c.sbuf([128, NK, 32], BF16, "wab"), "wab"), bar)
            gT = fresh(Tile(c.sbuf([16, S], F32, "gT"), "gT"), bar)
            ones16 = fresh(Tile(c.sbuf([16, 128], F32, "ones16"), "ones16"), bar)
            c.dma(c.pool, wab[:, :, :], wab_d, dst=wab)
            c.op(c.dve, lambda e: e.memset(ones16[:, :], 1.0), writes=[ones16])
            for tp in range(4):
                tsl = slice(tp * 512, (tp + 1) * 512)
                pb, pa = PS[2 * (tp % 2)], PS[2 * (tp % 2) + 1]
                for k in range(NK):
                    c.op(c.pe, lambda e: e.matmul(pb[0:16, :], lhsT=wab[:, k, 0:16], rhs=hn_sb[:, k, tsl],
                                                  start=(k == 0), stop=(k == NK - 1)),
                         reads=[wab, hn], writes=[pb], signal=(k == NK - 1))
                for k in range(NK):
                    c.op(c.pe, lambda e: e.matmul(pa[0:16, :], lhsT=wab[:, k, 16:32], rhs=hn_sb[:, k, tsl],
                                                  start=(k == 0), stop=(k == NK - 1)),
                         reads=[wab, hn], writes=[pa], signal=(k == NK - 1))
                c.op(c.act, lambda e: e.activation(out=bT[:, tsl], in_=pb[0:16, :], func=AF.Sigmoid),
                     reads=[pb], writes=[bT])
                c.op(c.act, lambda e: e.activation(out=gT[:, tsl], in_=pa[0:16, :], func=AF.Exp, bias=pv[:, 1:2], scale=1.0),
                     reads=[pa, pv], writes=[gT])
            c.op(c.dve, lambda e: e.tensor_scalar(out=gT[:, :], in0=gT[:, :], scalar1=1.0, scalar2=None, op0=ALU.add),
                 reads=[gT], writes=[gT])
            c.op(c.act, lambda e: e.activation(out=gT[:, :], in_=gT[:, :], func=AF.Ln), reads=[gT], writes=[gT])
            c.op(c.dve, lambda e: e.tensor_scalar(out=gT[:, :], in0=gT[:, :], scalar1=nA[:, 0:1], scalar2=None, op0=ALU.mult),
                 reads=[gT, nA], writes=[gT])
            for ch in range(16):
                csl = slice(ch * 128, (ch + 1) * 128)
                c.op(c.dve, lambda e: e.tensor_tensor_scan(out=gcT[:, csl], data0=ones16[:, :], data1=gT[:, csl],
                                                           initial=0.0, op0=ALU.mult, op1=ALU.add),
                     reads=[ones16, gT], writes=[gcT])
            for ch in range(16):
                csl = slice(ch * 128, (ch + 1) * 128)
                c.op(c.pe, lambda e: e.matmul(PS[4][:, ch * 16:(ch + 1) * 16], lhsT=gcT[:, csl], rhs=cst[0:16, 0, 0:16],
                                              start=True, stop=True), reads=[gcT, cst], writes=[PS[4]])
                c.op(c.pe, lambda e: e.matmul(PS[5][:, ch * 16:(ch + 1) * 16], lhsT=bT[:, csl], rhs=cst[0:16, 0, 0:16],
                                              start=True, stop=True), reads=[bT, cst], writes=[PS[5]])
            gcc = gc_col[:, :, :].rearrange("p a b -> p (a b)")
            bcc = b_col[:, :, :].rearrange("p a b -> p (a b)")
            c.op(c.dve, lambda e: e.tensor_copy(out=gcc, in_=PS[4][:, 0:256]), reads=[PS[4]], writes=[gc_col])
            c.op(c.dve, lambda e: e.tensor_copy(out=bcc, in_=PS[5][:, 0:256]), reads=[PS[5]], writes=[b_col])
            c.op(c.pe, lambda e: e.matmul(PS[6][:, 0:256], lhsT=cst[:, 4, :], rhs=gcc, start=True, stop=True),
                 reads=[cst, gc_col], writes=[PS[6]])
            dcc = dcol[:, :, :].rearrange("p a b -> p (a b)")
            becc = bec[:, :, :].rearrange("p a b -> p (a b)")
            nbcc = negb_col[:, :, :].rearrange("p a b -> p (a b)")
            c.op(c.dve, lambda e: e.tensor_tensor(out=dcc, in0=PS[6][:, 0:256], in1=gcc, op=ALU.subtract),
                 reads=[PS[6], gc_col], writes=[dcol])
            c.op(c.act, lambda e: e.activation(out=dcc, in_=dcc, func=AF.Exp), reads=[dcol], writes=[dcol])
            c.op(c.act, lambda e: e.activation(out=becc, in_=gcc, func=AF.Exp), reads=[gc_col], writes=[bec])
            c.op(c.dve, lambda e: e.tensor_tensor(out=becc, in0=becc, in1=bcc, op=ALU.mult), reads=[bec, b_col], writes=[bec])
            c.op(c.dve, lambda e: e.tensor_scalar(out=nbcc, in0=bcc, scalar1=-1.0, scalar2=None, op0=ALU.mult),
                 reads=[b_col], writes=[negb_col])
            c.stack = st
        bar = barrier_all(c)

        F = lambda shape, dt, name: fresh(Tile(c.sbuf(shape, dt, name), name), bar)
        wkh = F([128, NK, 768], BF16, "wkh")
        xpre = [F([128, 515], F32, f"xpre{i}") for i in range(4)]
        cacc = [F([128, 512], F32, f"cacc{i}") for i in range(2)]
        xs = [F([128, 512], F32, f"xs{i}") for i in range(2)]
        vT = [F([128, 512], BF16, f"vT{i}") for i in range(2)]
        sqb = F([128, 512], BF16, "sqb")
        rsd = F([128, 512], F32, "rsd")
        qhT = F([128, 512], BF16, "qhT")
        khT = F([128, 512], BF16, "khT")
        grow = F([128, 4, 128], F32, "grow")
        nbrow = F([128, 4, 128], F32, "nbrow")
        egrow = [F([128, 512], F32, f"egrow{i}") for i in range(2)]
        qeT = [F([128, 512], BF16, f"qeT{i}") for i in range(2)]
        ztmp = F([128, 256], F32, "ztmp")
        zsg = F([128, 4, 256], BF16, "zsg")
        X = F([128, 4, 128], F32, "X")
        Y = F([128, 4, 128], F32, "Y")
        E1 = F([128, 4, 128], F32, "E1")
        E2 = F([128, 4, 128], F32, "E2")
        DT = F([128, 4, 128], F32, "DT")
        Dm = F([128, 4, 128], F32, "Dm")
        DmT = F([128, 4, 128], F32, "DmT")
        Mt = [F([128, 4, 128], BF16, f"M{i}") for i in range(2)]
        Nt = [F([128, 4, 128], BF16, f"N{i}") for i in range(2)]
        Wt = [F([128, 4, 128], BF16, f"W{i}") for i in range(2)]
        Rk = F([128, 4, 128], BF16, "Rk")
        Rv = F([128, 4, 128], BF16, "Rv")
        kdec = [F([128, 4, 128], BF16, f"kdec{i}") for i in range(2)]
        u_sb = [F([128, 4, 128], F32, f"u{i}") for i in range(2)]
        wT = [F([128, 4, 128], BF16, f"wT{i}") for i in range(2)]
        AqkT = [F([128, 4, 128], BF16, f"Aqk{i}") for i in range(2)]
        selv = F([16, 128], F32, "selv")
        St = [F([128, 128], F32, f"S{i}") for i in range(2)]
        Sb = [F([128, 128], BF16, f"Sb{i}") for i in range(2)]
        vnew = [F([128, 128], BF16, f"vnew{i}") for i in range(2)]
        junk = F([128, 128], F32, "junk")
        sm = [F([128, 4], F32, f"sm{i}") for i in range(2)]
        ot = [F([128, 2, 128], BF16, f"ot{i}") for i in range(2)]

        def bc(ap3):
            return ap3.broadcast_to([128, 4, 128])

        def bcm(idx):
            return cst[:, idx:idx + 1, :].broadcast_to([128, 4, 128])

        v3 = lambda bank: bank[:, :].rearrange("p (a b) -> p a b", a=4)
        ocnt = 0
        for kh in range(NKH):
            c.dma(c.pool, wkh[:, :, :], wkh_d[kh], dst=wkh, max_dma_last_dim=8192)
            for vh in range(2):
                c.op(c.dve, lambda e: e.memset(St[vh][:, :], 0.0), writes=[St[vh]])
                c.op(c.dve, lambda e: e.memset(Sb[vh][:, :], 0.0), writes=[Sb[vh]])
            for qd in range(4):
                t0 = qd * 512
                tsl = slice(t0, t0 + 512)
                ch0 = qd * 4
                for gi in range(4):
                    bank = PS[gi % 2]
                    xp = xpre[gi]
                    for k in range(NK):
                        c.op(c.pe, lambda e: e.matmul(bank[:, :], lhsT=wkh[:, k, gi * 128:(gi + 1) * 128], rhs=hn_sb[:, k, tsl],
                                                      start=(k == 0), stop=(k == NK - 1)),
                             reads=[wkh, hn], writes=[bank], signal=(k == NK - 1))
                    if qd == 0:
                        c.op(c.dve, lambda e: e.memset(xp[:, 0:3], 0.0), writes=[xp])
                    else:
                        c.op(c.dve, lambda e: e.tensor_copy(out=xp[:, 0:3], in_=xp[:, 512:515]), reads=[xp], writes=[xp])
                    c.op(c.act, lambda e: e.activation(out=xp[:, 3:515], in_=bank[:, :], func=AF.Copy),
                         reads=[bank], writes=[xp])
                    acc = cacc[gi % 2]
                    c.op(c.dve, lambda e: e.tensor_scalar(out=acc[:, :], in0=xp[:, 3:515], scalar1=cw[:, kh, gi, 3:4],
                                                          scalar2=None, op0=ALU.mult), reads=[xp, cw], writes=[acc])
                    for j in (2, 1, 0):
                        c.op(c.dve, lambda e: e.scalar_tensor_tensor(
                            out=acc[:, :], in0=xp[:, j:j + 512], scalar=cw[:, kh, gi, j:j + 1], in1=acc[:, :],
                            op0=ALU.mult, op1=ALU.add), reads=[xp, cw, acc], writes=[acc])
                    dst = xs[gi] if gi < 2 else vT[gi - 2]
                    c.op(c.act, lambda e: e.activation(out=dst[:, :], in_=acc[:, :], func=AF.Silu),
                         reads=[acc], writes=[dst])
                for gi, dstT, cmul in ((0, qhT, 128 ** -0.5), (1, khT, 1.0)):
                    c.op(c.act, lambda e: e.activation(out=sqb[:, :], in_=xs[gi][:, :], func=AF.Square),
                         reads=[xs[gi]], writes=[sqb])
                    c.op(c.pe, lambda e: e.matmul(PS[2][:, :], lhsT=ones_bf[:, :], rhs=sqb[:, :], start=True, stop=True),
                         reads=[ones_bf, sqb], writes=[PS[2]])
                    c.op(c.dve, lambda e: e.tensor_scalar(out=rsd[:, :], in0=PS[2][:, :], scalar1=EPS, scalar2=None,
                                                          op0=ALU.add), reads=[PS[2]], writes=[rsd])
                    c.op(c.act, lambda e: e.activation(out=rsd[:, :], in_=rsd[:, :], func=AF.Sqrt), reads=[rsd], writes=[rsd])
                    c.op(c.dve, lambda e: e.reciprocal(out=rsd[:, :], in_=rsd[:, :]), reads=[rsd], writes=[rsd])
                    c.op(c.dve, lambda e: e.scalar_tensor_tensor(out=dstT[:, :], in0=xs[gi][:, :], scalar=cmul, in1=rsd[:, :],
                                                                 op0=ALU.mult, op1=ALU.mult),
                         reads=[xs[gi], rsd], writes=[dstT])
                for cc in range(4):
                    for k in range(NK):
                        c.op(c.pe, lambda e: e.matmul(PS[3][:, 0:256], lhsT=hn_sb[:, k, t0 + cc * 128:t0 + (cc + 1) * 128],
                                                      rhs=wkh[:, k, 512:768], start=(k == 0), stop=(k == NK - 1)),
                             reads=[hn, wkh], writes=[PS[3]], signal=(k == NK - 1))
                    c.op(c.act, lambda e: e.activation(out=ztmp[:, :], in_=PS[3][:, 0:256], func=AF.Silu),
                         reads=[PS[3]], writes=[ztmp])
                    c.op(c.dve, lambda e: e.tensor_tensor(out=zsg[:, cc, :], in0=ztmp[:, :], in1=ogr[:, :], op=ALU.mult),
                         reads=[ztmp, ogr], writes=[zsg])
                for cc in range(4):
                    csl = slice(cc * 128, (cc + 1) * 128)
                    c.op(c.pe, lambda e: e.matmul(PS[4][:, csl], lhsT=khT[:, csl], rhs=ident_bf[:, :], start=True, stop=True),
                         reads=[khT, ident_bf], writes=[PS[4]], signal=(cc == 3))
                for cc in range(4):
                    csl = slice(cc * 128, (cc + 1) * 128)
                    c.op(c.pe, lambda e: e.matmul(PS[6][:, csl], lhsT=khT[:, csl], rhs=khT[:, csl], start=True, stop=True),
                         reads=[khT], writes=[PS[6]], signal=(cc == 3))
                for cc in range(4):
                    csl = slice(cc * 128, (cc + 1) * 128)
                    c.op(c.pe, lambda e: e.matmul(PS[7][:, csl], lhsT=khT[:, csl], rhs=qhT[:, csl], start=True, stop=True),
                         reads=[khT, qhT], writes=[PS[7]], signal=(cc == 3))
                for vh in range(2):
                    lv = 2 * kh + vh
                    colv = lambda t: t[:, ch0:ch0 + 4, lv:lv + 1]
                    c.op(c.dve, lambda e: e.tensor_tensor(out=kdec[vh][:, :, :], in0=v3(PS[4]), in1=bc(colv(dcol)), op=ALU.mult),
                         reads=[PS[4], dcol], writes=[kdec[vh]])
                    c.op(c.dve, lambda e: e.tensor_tensor(out=Rk[:, :, :], in0=v3(PS[4]), in1=bc(colv(bec)), op=ALU.mult),
                         reads=[PS[4], bec], writes=[Rk])
                    for cc in range(4):
                        csl = slice(cc * 128, (cc + 1) * 128)
                        c.op(c.pe, lambda e: e.matmul(PS[5][:, csl], lhsT=vT[vh][:, csl], rhs=ident_bf[:, :], start=True, stop=True),
                             reads=[vT[vh], ident_bf], writes=[PS[5]], signal=(cc == 3))
                    c.op(c.dve, lambda e: e.tensor_tensor(out=Rv[:, :, :], in0=v3(PS[5]), in1=bc(colv(b_col)), op=ALU.mult),
                         reads=[PS[5], b_col], writes=[Rv])
                    c.op(c.dve, lambda e: e.tensor_copy(out=selv[:, :], in_=cst[0:16, 0, lv:lv + 1].broadcast_to([16, 128])),
                         reads=[cst], writes=[selv])
                    c.op(c.pe, lambda e: e.matmul(PS[0][:, :], lhsT=selv[:, :], rhs=gcT[:, tsl], start=True, stop=True),
                         reads=[selv, gcT], writes=[PS[0]])
                    c.op(c.pe, lambda e: e.matmul(PS[1][:, :], lhsT=selv[:, :], rhs=bT[:, tsl], start=True, stop=True),
                         reads=[selv, bT], writes=[PS[1]])
                    g2 = grow[:, :, :].rearrange("p a b -> p (a b)")
                    c.op(c.act, lambda e: e.activation(out=g2, in_=PS[0][:, :], func=AF.Copy), reads=[PS[0]], writes=[grow])
                    c.op(c.act, lambda e: e.activation(out=egrow[vh][:, :], in_=PS[0][:, :], func=AF.Exp),
                         reads=[PS[0]], writes=[egrow[vh]])
                    c.op(c.dve, lambda e: e.tensor_scalar(out=nbrow[:, :, :].rearrange("p a b -> p (a b)"), in0=PS[1][:, :],
                                                          scalar1=-1.0, scalar2=None, op0=ALU.mult),
                         reads=[PS[1]], writes=[nbrow])
                    c.op(c.dve, lambda e: e.tensor_tensor(out=qeT[vh][:, :], in0=qhT[:, :], in1=egrow[vh][:, :], op=ALU.mult),
                         reads=[qhT, egrow[vh]], writes=[qeT[vh]])
                    c.op(c.dve, lambda e: e.tensor_tensor(out=X[:, :, :], in0=grow[:, :, :], in1=bc(colv(gc_col)), op=ALU.subtract),
                         reads=[grow, gc_col], writes=[X])
                    c.op(c.dve, lambda e: e.tensor_scalar(out=Y[:, :, :], in0=X[:, :, :], scalar1=0.0, scalar2=None, op0=ALU.min),
                         reads=[X], writes=[Y])
                    c.op(c.act, lambda e: e.activation(out=E1[:, :, :], in_=Y[:, :, :], func=AF.Exp), reads=[Y], writes=[E1])
                    c.op(c.dve, lambda e: e.tensor_scalar(out=Y[:, :, :], in0=X[:, :, :], scalar1=0.0, scalar2=None, op0=ALU.max),
                         reads=[X], writes=[Y])
                    c.op(c.act, lambda e: e.activation(out=E2[:, :, :], in_=Y[:, :, :], func=AF.Exp, scale=-1.0),
                         reads=[Y], writes=[E2])
                    c.op(c.dve, lambda e: e.tensor_tensor(out=DT[:, :, :], in0=E1[:, :, :], in1=bcm(1), op=ALU.mult),
                         reads=[E1, cst], writes=[DT])
                    c.op(c.dve, lambda e: e.tensor_tensor(out=DmT[:, :, :], in0=E1[:, :, :], in1=nbrow[:, :, :], op=ALU.mult),
                         reads=[E1, nbrow], writes=[DmT])
                    c.op(c.dve, lambda e: e.tensor_tensor(out=DmT[:, :, :], in0=DmT[:, :, :], in1=bcm(2), op=ALU.mult),
                         reads=[DmT, cst], writes=[DmT])
                    c.op(c.dve, lambda e: e.tensor_tensor(out=Dm[:, :, :], in0=E2[:, :, :], in1=bc(colv(negb_col)), op=ALU.mult),
                         reads=[E2, negb_col], writes=[Dm])
                    c.op(c.dve, lambda e: e.tensor_tensor(out=Dm[:, :, :], in0=Dm[:, :, :], in1=bcm(3), op=ALU.mult),
                         reads=[Dm, cst], writes=[Dm])
                    c.op(c.dve, lambda e: e.tensor_tensor(out=AqkT[vh][:, :, :], in0=v3(PS[7]), in1=DT[:, :, :], op=ALU.mult),
                         reads=[PS[7], DT], writes=[AqkT[vh]])
                    c.op(c.dve, lambda e: e.tensor_tensor(out=Mt[0][:, :, :], in0=v3(PS[6]), in1=Dm[:, :, :], op=ALU.mult),
                         reads=[PS[6], Dm], writes=[Mt[0]])
                    c.op(c.dve, lambda e: e.tensor_tensor(out=Nt[0][:, :, :], in0=v3(PS[6]), in1=DmT[:, :, :], op=ALU.mult),
                         reads=[PS[6], DmT], writes=[Nt[0]])
                    c.op(c.dve, lambda e: e.tensor_tensor(out=Wt[0][:, :, :], in0=Nt[0][:, :, :], in1=bcm(0), op=ALU.add),
                         reads=[Nt[0], cst], writes=[Wt[0]])
                    cur = 0
                    for kk in range(1, 7):
                        Mc, Nc, Wc = Mt[cur], Nt[cur], Wt[cur]
                        Mn, Nn, Wn = Mt[1 - cur], Nt[1 - cur], Wt[1 - cur]
                        for cc in range(4):
                            csl = slice(cc * 128, (cc + 1) * 128)
                            c.op(c.pe, lambda e: e.matmul(PS[0][:, csl], lhsT=Nc[:, cc, :], rhs=Mc[:, cc, :], start=True, stop=True),
                                 reads=[Nc, Mc], writes=[PS[0]], signal=(cc == 3))
                        if kk < 6:
                            for cc in range(4):
                                csl = slice(cc * 128, (cc + 1) * 128)
                                c.op(c.pe, lambda e: e.matmul(PS[1][:, csl], lhsT=Mc[:, cc, :], rhs=Nc[:, cc, :], start=True, stop=True),
                                     reads=[Nc, Mc], writes=[PS[1]], signal=(cc == 3))
                        c.op(c.act, lambda e: e.activation(out=Mn[:, :, :], in_=v3(PS[0]), func=AF.Copy),
                             reads=[PS[0]], writes=[Mn])
                        if kk < 6:
                            c.op(c.dve, lambda e: e.tensor_copy(out=Nn[:, :, :], in_=v3(PS[1])), reads=[PS[1]], writes=[Nn])
                        for cc in range(4):
                            csl = slice(cc * 128, (cc + 1) * 128)
                            c.op(c.pe, lambda e: e.matmul(PS[2][:, csl], lhsT=Mn[:, cc, :], rhs=Wc[:, cc, :], start=True, stop=True),
                                 reads=[Mn, Wc], writes=[PS[2]], signal=(cc == 3))
                        c.op(c.dve, lambda e: e.tensor_tensor(out=Wn[:, :, :], in0=Wc[:, :, :], in1=v3(PS[2]), op=ALU.add),
                             reads=[Wc, PS[2]], writes=[Wn])
                        cur = 1 - cur
                    TT = Wt[cur]
                    for cc in range(4):
                        csl = slice(cc * 128, (cc + 1) * 128)
                        c.op(c.pe, lambda e: e.matmul(PS[3][:, csl], lhsT=TT[:, cc, :], rhs=Rv[:, cc, :], start=True, stop=True),
                             reads=[TT, Rv], writes=[PS[3]], signal=(cc == 3))
                    c.op(c.act, lambda e: e.activation(out=u_sb[vh][:, :, :], in_=v3(PS[3]), func=AF.Copy),
                         reads=[PS[3]], writes=[u_sb[vh]])
                    for cc in range(4):
                        csl = slice(cc * 128, (cc + 1) * 128)
                        c.op(c.pe, lambda e: e.matmul(PS[5][:, csl], lhsT=Rk[:, cc, :], rhs=TT[:, cc, :], start=True, stop=True),
                             reads=[TT, Rk], writes=[PS[5]], signal=(cc == 3))
                    c.op(c.dve, lambda e: e.tensor_copy(out=wT[vh][:, :, :], in_=v3(PS[5])), reads=[PS[5]], writes=[wT[vh]])
                for cc in range(4):
                    csl = slice(cc * 128, (cc + 1) * 128)
                    o_ = ot[ocnt % 2]
                    ocnt += 1
                    for vh in range(2):
                        lv = 2 * kh + vh
                        pb = PS[6 + vh]
                        c.op(c.pe, lambda e: e.matmul(pb[:, 0:128], lhsT=wT[vh][:, cc, :], rhs=Sb[vh][:, :], start=True, stop=True),
                             reads=[wT[vh], Sb[vh]], writes=[pb])
                        c.op(c.dve, lambda e: e.tensor_tensor(out=vnew[vh][:, :], in0=u_sb[vh][:, cc, :], in1=pb[:, 0:128],
                                                              op=ALU.subtract), reads=[u_sb[vh], pb], writes=[vnew[vh]])
                        c.op(c.pe, lambda e: e.matmul(pb[:, 128:256], lhsT=qeT[vh][:, csl], rhs=Sb[vh][:, :], start=True, stop=False),
                             reads=[qeT[vh], Sb[vh]], writes=[pb], signal=False)
                        c.op(c.pe, lambda e: e.matmul(pb[:, 128:256], lhsT=AqkT[vh][:, cc, :], rhs=vnew[vh][:, :], start=False, stop=True),
                             reads=[AqkT[vh], vnew[vh]], writes=[pb], signal=False)
                        c.op(c.pe, lambda e: e.matmul(pb[:, 256:384], lhsT=kdec[vh][:, cc, :], rhs=vnew[vh][:, :], start=True, stop=True),
                             reads=[kdec[vh], vnew[vh]], writes=[pb])
                        egl = egrow[vh][:, cc * 128 + 127:cc * 128 + 128]
                        c.op(c.dve, lambda e: e.scalar_tensor_tensor(out=St[vh][:, :], in0=St[vh][:, :], scalar=egl, in1=pb[:, 256:384],
                                                                     op0=ALU.mult, op1=ALU.add),
                             reads=[St[vh], egrow[vh], pb], writes=[St[vh]])
                        c.op(c.act, lambda e: e.activation(out=Sb[vh][:, :], in_=St[vh][:, :], func=AF.Copy),
                             reads=[St[vh]], writes=[Sb[vh]])
                        c.op(c.act, lambda e: e.activation(out=junk[:, :], in_=pb[:, 128:256], func=AF.Square),
                             reads=[pb], writes=[junk])
                        c.op(c.dve, lambda e: e.reduce_sum(out=sm[vh][:, 0:1], in_=junk[:, :], axis=AX.X),
                             reads=[junk], writes=[sm[vh]])
                        c.op(c.dve, lambda e: e.tensor_scalar(out=sm[vh][:, 1:2], in0=sm[vh][:, 0:1], scalar1=1.0 / 128, scalar2=EPS,
                                                              op0=ALU.mult, op1=ALU.add), reads=[sm[vh]], writes=[sm[vh]])
                        c.op(c.act, lambda e: e.activation(out=sm[vh][:, 2:3], in_=sm[vh][:, 1:2], func=AF.Sqrt),
                             reads=[sm[vh]], writes=[sm[vh]])
                        c.op(c.dve, lambda e: e.reciprocal(out=sm[vh][:, 3:4], in_=sm[vh][:, 2:3]), reads=[sm[vh]], writes=[sm[vh]])
                        c.op(c.dve, lambda e: e.scalar_tensor_tensor(out=o_[:, vh, :], in0=pb[:, 128:256], scalar=sm[vh][:, 3:4],
                                                                     in1=zsg[:, cc, vh * 128:(vh + 1) * 128],
                                                                     op0=ALU.mult, op1=ALU.mult),
                             reads=[pb, sm[vh], zsg], writes=[o_])
                    c.dma(c.sp, out_d[:, ch0 + cc, 2 * kh:2 * kh + 2, :], o_[:, :, :], dst=out_t, src=o_)
        c.finish([out_t])
    return nc


def gdn_consts():
    I = np.eye(128, dtype=np.float32)
    U = np.triu(np.ones((128, 128), np.float32))
    sU = np.triu(np.ones((128, 128), np.float32), 1)
    sL = np.tril(np.ones((128, 128), np.float32), -1)
    s127 = np.zeros((128, 128), np.float32)
    s127[127, :] = 1.0
    return np.ascontiguousarray(np.stack([I, U, sU, sL, s127], axis=1))


def lay_wcols(w):
    return np.ascontiguousarray(w.reshape(NK, 128, -1).transpose(1, 0, 2))


def gdn_weights(w_in, conv_w, a_log, dt_bias, o_gain, g_mix, hh):
    KW, VW = 2048, 4096
    qs, ks, vs, zs = 0, KW, 2 * KW, 2 * KW + VW
    bs = 2 * KW + 2 * VW
    as_ = bs + 32
    wkh = []
    cw = np.zeros((128, NKH, 4, 4), np.float32)
    for kh in range(NKH):
        gk = hh * NKH + kh
        cols = np.concatenate([
            np.arange(qs + gk * 128, qs + (gk + 1) * 128),
            np.arange(ks + gk * 128, ks + (gk + 1) * 128),
            np.arange(vs + 2 * gk * 128, vs + (2 * gk + 2) * 128),
            np.arange(zs + 2 * gk * 128, zs + (2 * gk + 2) * 128)])
        wkh.append(lay_wcols(w_in[:, cols]))
        for gi, c0 in enumerate((qs + gk * 128, ks + gk * 128, vs + 2 * gk * 128, vs + (2 * gk + 1) * 128)):
            cw[:, kh, gi, :] = conv_w[:, c0:c0 + 128].T
    lvs = np.arange(hh * NVH, (hh + 1) * NVH)
    wab = lay_wcols(np.concatenate([w_in[:, bs + lvs], w_in[:, as_ + lvs]], axis=1))
    return {
        "g": np.ascontiguousarray(g_mix.reshape(NK, 128).T),
        "wkh": np.stack(wkh), "wab": wab, "cw": cw,
        "pv": np.ascontiguousarray(np.stack([a_log[lvs], dt_bias[lvs]], axis=1)),
        "ogr": np.ascontiguousarray(np.broadcast_to(np.concatenate([o_gain, o_gain])[None, :], (128, 256))),
        "cst": gdn_consts(),
    }


_PROGS = {}


def _prog(name):
    if name not in _PROGS:
        if name == "fox":
            _PROGS[name] = build_fox()
        elif name == "gdn":
            _PROGS[name] = build_gdn()
        elif name == "dense":
            _PROGS[name] = build_ffn(2048, 1, 5632)
        elif name == "moe":
            _PROGS[name] = build_ffn(4096, 8, 2816)
    return _PROGS[name]


def _run(name, in_maps):
    res = run_bass_kernel_spmd(_prog(name), in_maps, core_ids=list(range(8)))
    return [r["out"] for r in res.results]


def kernel(x, norm_mix, norm_ffn, fox_w_in, fox_b_f, fox_q_norm, fox_k_norm, fox_w_out,
           gdn_w_in, gdn_conv, gdn_a_log, gdn_dt_bias, gdn_o_norm, gdn_w_out,
           ffn_w_gate, ffn_w_up, ffn_w_down, moe_router, moe_w_gate, moe_w_up, moe_w_down):
    f32 = lambda a: np.asarray(a, dtype=np.float32)
    h = f32(x)
    B = h.shape[0]
    for i in range(4):
        j = i // 2
        hT_b = [lay_hT(h[b]) for b in range(B)]
        if i % 2 == 0:
            in_maps = []
            for c in range(8):
                b, hh = c // 2, c % 2
                d = fox_weights(f32(fox_w_in[j]), f32(fox_b_f[j]), f32(fox_q_norm[j]), f32(fox_k_norm[j]),
                                f32(norm_mix[i]), hh)
                d["hT"] = hT_b[b]
                in_maps.append(d)
            outs = _run("fox", in_maps)
            oT = []
            for c in range(8):
                b, half = c // 2, c % 2
                tsl = slice(half * T, (half + 1) * T)
                oT.append(np.ascontiguousarray(np.concatenate(
                    [outs[2 * b][:, :, tsl], outs[2 * b + 1][:, :, tsl]], axis=1)))
            wts = ffn_weights(f32(fox_w_out[j]), f32(norm_ffn[i]), f32(ffn_w_gate[j])[None], f32(ffn_w_up[j])[None],
                              f32(ffn_w_down[j])[None])
            name = "dense"
        else:
            in_maps = []
            for c in range(8):
                b, hh = c // 2, c % 2
                d = gdn_weights(f32(gdn_w_in[j]), f32(gdn_conv[j]), f32(gdn_a_log[j]), f32(gdn_dt_bias[j]),
                                f32(gdn_o_norm[j]), f32(norm_mix[i]), hh)
                d["hT"] = hT_b[b]
                in_maps.append(d)
            outs = _run("gdn", in_maps)
            oT = []
            for c in range(8):
                b, half = c // 2, c % 2
                tsl = slice(half * T, (half + 1) * T)
                both = np.concatenate([outs[2 * b][tsl], outs[2 * b + 1][tsl]], axis=1)
                oT.append(np.ascontiguousarray(both.transpose(2, 1, 0)))
            wts = ffn_weights(f32(gdn_w_out[j]), f32(norm_ffn[i]), f32(moe_w_gate[j]), f32(moe_w_up[j]),
                              f32(moe_w_down[j]), f32(moe_router[j]))
            name = "moe"
        hflat = h.reshape(B * S, D)
        in_maps = []
        for c in range(8):
            d = dict(wts)
            d["hT"] = lay_feat_major(hflat[c * T:(c + 1) * T])
            d["oT"] = oT[c]
            in_maps.append(d)
        outs = _run(name, in_maps)
        h = np.concatenate([unlay_feat_major(o) for o in outs], axis=0).reshape(B, S, D)
    return np.ascontiguousarray(h.astype(np.float32))
```
